# Optimizing a Trainium2 kernel written in Bass

```python
import math
import jax, jax.numpy as jnp
from jax import lax
import numpy as np

D_MODEL = 2048
BATCH = 8
SEQ = 2048
DEPTH = 2

MLSTM_HEADS = 8
MLSTM_DH = 128
MLSTM_W = MLSTM_HEADS * MLSTM_DH
MLSTM_CHUNK = 64
QK_CONV = 4
NSA_HEADS = 8
NSA_KV_HEADS = 2
NSA_GROUP = NSA_HEADS // NSA_KV_HEADS
NSA_DH = 128
NSA_W = NSA_HEADS * NSA_DH
NSA_KV_W = NSA_KV_HEADS * NSA_DH
CMP_BLOCK = 32
CMP_STRIDE = 16
CMP_HIDDEN = 256
SEL_BLOCK = 64
SEL_TOPK = 16
SEL_QCHUNK = 64
WINDOW = 512
WIN_QBLOCK = 128
ROPE_THETA = 500000.0
ROPE_DIM = NSA_DH // 4
D_FF = 5632
FFN_CONV = 3
NORM_EPS = 1e-6
NEG = -1e30
FORCE_SCORE = 1e4

SPLIT_SIZES = (MLSTM_W, MLSTM_W, MLSTM_W, MLSTM_W, MLSTM_HEADS, MLSTM_HEADS,
               NSA_W, NSA_KV_W, NSA_KV_W, NSA_KV_W, NSA_KV_W, NSA_KV_W, NSA_KV_W,
               3 * NSA_HEADS)
N_IN = 4 * MLSTM_W + 2 * MLSTM_HEADS + NSA_W + 6 * NSA_KV_W + 3 * NSA_HEADS

kernel_name = "hymba_mlstm_nsa_convffn"


def rmsnorm(x, g):
    xf = x.astype(jnp.float32)
    y = xf * lax.rsqrt(jnp.mean(xf * xf, axis=-1, keepdims=True) + NORM_EPS)
    return (y * g.astype(jnp.float32)).astype(x.dtype)


def split_cols(z):
    pieces, start = [], 0
    for size in SPLIT_SIZES:
        pieces.append(z[..., start:start + size])
        start += size
    return pieces


def causal_dwconv(x, w, b):
    K, S = w.shape[0], x.shape[1]
    xp = jnp.pad(x, ((0, 0), (K - 1, 0), (0, 0)))
    y = b
    for j in range(K):
        y = y + xp[:, j:j + S] * w[j]
    return y


def rope_tables(seq):
    pos = jnp.arange(seq, dtype=jnp.float32)
    inv = ROPE_THETA ** (-jnp.arange(0, ROPE_DIM, 2, dtype=jnp.float32) / ROPE_DIM)
    ang = pos[:, None] * inv[None, :]
    return jnp.cos(ang), jnp.sin(ang)


def partial_rope(x, cos, sin):
    half = ROPE_DIM // 2
    c = cos[None, :, None, :].astype(x.dtype)
    s = sin[None, :, None, :].astype(x.dtype)
    x1, x2, rest = x[..., :half], x[..., half:ROPE_DIM], x[..., ROPE_DIM:]
    return jnp.concatenate([x1 * c - x2 * s, x2 * c + x1 * s, rest], axis=-1)


def mlstm_chunkwise(q, k, v, i_pre, f_pre):
    B, H, S, d = q.shape
    L = MLSTM_CHUNK
    nc = S // L
    qf = q.astype(jnp.float32)
    kf = k.astype(jnp.float32) * (d ** -0.5)
    vf = v.astype(jnp.float32)
    logf = jax.nn.log_sigmoid(f_pre)

    def chunks(a):
        return jnp.moveaxis(a.reshape((B, H, nc, L) + a.shape[3:]), 2, 0)

    causal = jnp.tril(jnp.ones((L, L), dtype=bool))

    def step(carry, inp):
        C, n, m = carry
        q_, k_, v_, i_, lf = inp
        b = jnp.cumsum(lf, axis=-1)
        a = b + m[..., None]
        D = b[..., :, None] - b[..., None, :] + i_[..., None, :]
        D = jnp.where(causal, D, -jnp.inf)
        m_t = jnp.maximum(a, jnp.max(D, axis=-1))
        w_inter = jnp.exp(a - m_t)
        W = jnp.exp(D - m_t[..., None])
        s_qk = jnp.einsum('bhtd,bhsd->bhts', q_, k_) * W
        num = (w_inter[..., None] * jnp.einsum('bhtd,bhvd->bhtv', q_, C)
               + jnp.einsum('bhts,bhsv->bhtv', s_qk, v_))
        den = w_inter * jnp.einsum('bhtd,bhd->bht', q_, n) + jnp.sum(s_qk, axis=-1)
        h = num / jnp.maximum(jnp.abs(den), jnp.exp(-m_t))[..., None]
        bL = b[..., -1]
        g = bL[..., None] - b + i_
        m_new = jnp.maximum(bL + m, jnp.max(g, axis=-1))
        decay = jnp.exp(bL + m - m_new)
        wk = jnp.exp(g - m_new[..., None])
        C_new = decay[..., None, None] * C + jnp.einsum('bhs,bhsv,bhsd->bhvd', wk, v_, k_)
        n_new = decay[..., None] * n + jnp.einsum('bhs,bhsd->bhd', wk, k_)
        return (C_new, n_new, m_new), h

    init = (jnp.zeros((B, H, d, d), jnp.float32), jnp.zeros((B, H, d), jnp.float32),
            jnp.zeros((B, H), jnp.float32))
    _, hc = lax.scan(step, init, (chunks(qf), chunks(kf), chunks(vf), chunks(i_pre), chunks(logf)))
    return jnp.moveaxis(hc, 0, 2).reshape(B, H, S, d)


def compress_blocks(k, pe, w1, w2):
    B, Hk, S, d = k.shape
    ncmp = (S - CMP_BLOCK) // CMP_STRIDE + 1
    idx = jnp.arange(ncmp)[:, None] * CMP_STRIDE + jnp.arange(CMP_BLOCK)[None, :]
    blk = k[:, :, idx] + pe.astype(k.dtype)
    blk = blk.reshape(B, Hk, ncmp, CMP_BLOCK * d)
    return jax.nn.gelu(blk @ w1) @ w2


def nsa_group(q, k_cmp, v_cmp, k_slc, v_slc, k_win, v_win, gate_pre,
              cmp_pe_k, cmp_pe_v, cmp_w1_k, cmp_w2_k, cmp_w1_v, cmp_w2_v):
    B, S, _, d = q.shape
    scale = d ** -0.5
    pos = jnp.arange(S)
    q5 = q.reshape(B, S, NSA_KV_HEADS, NSA_GROUP, d).transpose(0, 2, 3, 1, 4)
    heads = lambda a: a.transpose(0, 2, 1, 3)
    k_cmp, v_cmp, k_slc, v_slc, k_win, v_win = map(heads, (k_cmp, v_cmp, k_slc, v_slc, k_win, v_win))

    kc = compress_blocks(k_cmp, cmp_pe_k, cmp_w1_k, cmp_w2_k)
    vc = compress_blocks(v_cmp, cmp_pe_v, cmp_w1_v, cmp_w2_v)
    ncmp = kc.shape[2]
    block_end = jnp.arange(ncmp) * CMP_STRIDE + CMP_BLOCK - 1
    valid_c = block_end[None, :] <= pos[:, None]
    sc = jnp.einsum('bhgsd,bhcd->bhgsc', q5, kc).astype(jnp.float32) * scale
    p_cmp = jax.nn.softmax(jnp.where(valid_c, sc, NEG), axis=-1) * valid_c
    o_cmp = jnp.einsum('bhgsc,bhcd->bhgsd', p_cmp.astype(vc.dtype), vc)

    nsel = S // SEL_BLOCK
    n_top = min(SEL_TOPK, nsel)
    jb = jnp.arange(nsel)
    cstart = jnp.arange(ncmp) * CMP_STRIDE
    overlap = ((cstart[:, None] < (jb[None, :] + 1) * SEL_BLOCK)
               & (cstart[:, None] + CMP_BLOCK > jb[None, :] * SEL_BLOCK)).astype(jnp.float32)
    imp = jnp.einsum('bhgsc,cj->bhsj', p_cmp, overlap)
    cur = pos // SEL_BLOCK
    forced = (jb[None, :] == 0) | (jb[None, :] == cur[:, None]) | (jb[None, :] == cur[:, None] - 1)
    imp = jnp.where(forced, FORCE_SCORE, imp)
    imp = jnp.where(jb[None, :] <= cur[:, None], imp, -1.0)
    _, sel_idx = lax.top_k(imp, n_top)

    kb = k_slc.reshape(B, NSA_KV_HEADS, nsel, SEL_BLOCK, d)
    vb = v_slc.reshape(B, NSA_KV_HEADS, nsel, SEL_BLOCK, d)
    gather = jax.vmap(jax.vmap(lambda blocks, idx: blocks[idx]))
    QC = SEL_QCHUNK

    def sel_chunk(c):
        s0 = c * QC
        qc = lax.dynamic_slice_in_dim(q5, s0, QC, axis=3)
        ic = lax.dynamic_slice_in_dim(sel_idx, s0, QC, axis=2)
        ks = gather(kb, ic).reshape(B, NSA_KV_HEADS, QC, n_top * SEL_BLOCK, d)
        vs = gather(vb, ic).reshape(B, NSA_KV_HEADS, QC, n_top * SEL_BLOCK, d)
        kpos = (ic[..., None] * SEL_BLOCK + jnp.arange(SEL_BLOCK)).reshape(B, NSA_KV_HEADS, QC, n_top * SEL_BLOCK)
        qpos = s0 + jnp.arange(QC)
        mask = kpos <= qpos[None, None, :, None]
        s = jnp.einsum('bhgqd,bhqkd->bhgqk', qc, ks).astype(jnp.float32) * scale
        p = jax.nn.softmax(jnp.where(mask[:, :, None], s, NEG), axis=-1)
        return jnp.einsum('bhgqk,bhqkd->bhgqd', p.astype(vs.dtype), vs)

    o_slc = lax.map(sel_chunk, jnp.arange(S // QC))
    o_slc = jnp.moveaxis(o_slc, 0, 3).reshape(B, NSA_KV_HEADS, NSA_GROUP, S, d)

    QB, KW = WIN_QBLOCK, WINDOW + WIN_QBLOCK
    kp = jnp.pad(k_win, ((0, 0), (0, 0), (WINDOW, 0), (0, 0)))
    vp = jnp.pad(v_win, ((0, 0), (0, 0), (WINDOW, 0), (0, 0)))

    def win_block(c):
        s0 = c * QB
        qb = lax.dynamic_slice_in_dim(q5, s0, QB, axis=3)
        kw = lax.dynamic_slice_in_dim(kp, s0, KW, axis=2)
        vw = lax.dynamic_slice_in_dim(vp, s0, KW, axis=2)
        kpos = s0 - WINDOW + jnp.arange(KW)
        diff = (s0 + jnp.arange(QB))[:, None] - kpos[None, :]
        mask = (kpos[None, :] >= 0) & (diff >= 0) & (diff < WINDOW)
        s = jnp.einsum('bhgqd,bhkd->bhgqk', qb, kw).astype(jnp.float32) * scale
        p = jax.nn.softmax(jnp.where(mask, s, NEG), axis=-1)
        return jnp.einsum('bhgqk,bhkd->bhgqd', p.astype(vw.dtype), vw)

    o_win = lax.map(win_block, jnp.arange(S // QB))
    o_win = jnp.moveaxis(o_win, 0, 3).reshape(B, NSA_KV_HEADS, NSA_GROUP, S, d)

    g = jax.nn.sigmoid(gate_pre.astype(jnp.float32)).reshape(B, S, NSA_KV_HEADS, NSA_GROUP, 3)
    g = g.transpose(0, 2, 3, 1, 4).astype(q.dtype)
    o = g[..., 0:1] * o_cmp + g[..., 1:2] * o_slc + g[..., 2:3] * o_win
    return o.transpose(0, 3, 1, 2, 4).reshape(B, S, NSA_W)


def hybrid_mixer(h, w_in, qk_conv_w, qk_conv_b, i_bias, f_bias, mlstm_norm,
                 cmp_pe_k, cmp_pe_v, cmp_w1_k, cmp_w2_k, cmp_w1_v, cmp_w2_v,
                 nsa_norm, w_out, cos, sin):
    B, S, _ = h.shape
    (mq, mk, mv, mo, mi, mf, nq, kc, vc, ks, vs, kw, vw, ng) = split_cols(h @ w_in)

    qk = jax.nn.silu(causal_dwconv(jnp.concatenate([mq, mk], axis=-1), qk_conv_w, qk_conv_b))
    mq, mk = qk[..., :MLSTM_W], qk[..., MLSTM_W:]
    to_h = lambda a: a.reshape(B, S, MLSTM_HEADS, MLSTM_DH).transpose(0, 2, 1, 3)
    i_pre = (mi.astype(jnp.float32) + i_bias.astype(jnp.float32)).transpose(0, 2, 1)
    f_pre = (mf.astype(jnp.float32) + f_bias.astype(jnp.float32)).transpose(0, 2, 1)
    hm = mlstm_chunkwise(to_h(mq), to_h(mk), to_h(mv), i_pre, f_pre).transpose(0, 2, 1, 3)
    hm = rmsnorm(hm, mlstm_norm.reshape(MLSTM_HEADS, MLSTM_DH)).reshape(B, S, MLSTM_W)
    mix_a = hm.astype(h.dtype) * jax.nn.sigmoid(mo)

    q = partial_rope(nq.reshape(B, S, NSA_HEADS, NSA_DH), cos, sin)
    kv = lambda a: a.reshape(B, S, NSA_KV_HEADS, NSA_DH)
    rk = lambda a: partial_rope(kv(a), cos, sin)
    mix_b = nsa_group(q, rk(kc), kv(vc), rk(ks), kv(vs), rk(kw), kv(vw), ng,
                      cmp_pe_k, cmp_pe_v, cmp_w1_k, cmp_w2_k, cmp_w1_v, cmp_w2_v)
    mix_b = rmsnorm(mix_b, nsa_norm)

    return jnp.concatenate([mix_a, mix_b], axis=-1) @ w_out


def conv_gated_mlp(h, w_up, conv_w, conv_b, w_down):
    u = causal_dwconv(h @ w_up, conv_w, conv_b)
    gate, up = u[..., :D_FF], u[..., D_FF:]
    return (jax.nn.silu(gate) * up) @ w_down


def setup_inputs(seed: int = 0) -> dict:
    key = jax.random.key(seed)
    ks = jax.random.split(key, 32)
    f32 = jnp.float32
    nrm = lambda k, shape, fan_in: jax.random.normal(k, shape, f32) * (fan_in ** -0.5)
    gain = lambda k, shape: 1.0 + 0.02 * jax.random.normal(k, shape, f32)
    small = lambda k, shape, s: s * jax.random.normal(k, shape, f32)
    return {
        "x": jax.random.normal(ks[0], (BATCH, SEQ, D_MODEL), f32),
        "attn_norm": gain(ks[1], (DEPTH, D_MODEL)),
        "w_in": nrm(ks[2], (DEPTH, D_MODEL, N_IN), D_MODEL),
        "qk_conv_w": nrm(ks[3], (DEPTH, QK_CONV, 2 * MLSTM_W), QK_CONV),
        "qk_conv_b": small(ks[4], (DEPTH, 2 * MLSTM_W), 0.02),
        "i_bias": small(ks[5], (DEPTH, MLSTM_HEADS), 0.1),
        "f_bias": 3.0 + 3.0 * jax.random.uniform(ks[6], (DEPTH, MLSTM_HEADS), f32),
        "mlstm_norm": gain(ks[7], (DEPTH, MLSTM_W)),
        "cmp_pe_k": small(ks[8], (DEPTH, CMP_BLOCK, NSA_DH), 0.1),
        "cmp_pe_v": small(ks[9], (DEPTH, CMP_BLOCK, NSA_DH), 0.1),
        "cmp_w1_k": nrm(ks[10], (DEPTH, CMP_BLOCK * NSA_DH, CMP_HIDDEN), CMP_BLOCK * NSA_DH),
        "cmp_w2_k": nrm(ks[11], (DEPTH, CMP_HIDDEN, NSA_DH), CMP_HIDDEN),
        "cmp_w1_v": nrm(ks[12], (DEPTH, CMP_BLOCK * NSA_DH, CMP_HIDDEN), CMP_BLOCK * NSA_DH),
        "cmp_w2_v": nrm(ks[13], (DEPTH, CMP_HIDDEN, NSA_DH), CMP_HIDDEN),
        "nsa_norm": gain(ks[14], (DEPTH, NSA_W)),
        "w_out": nrm(ks[15], (DEPTH, D_MODEL, D_MODEL), D_MODEL),
        "ffn_norm": gain(ks[16], (DEPTH, D_MODEL)),
        "w_up": nrm(ks[17], (DEPTH, D_MODEL, 2 * D_FF), D_MODEL),
        "ffn_conv_w": nrm(ks[18], (DEPTH, FFN_CONV, 2 * D_FF), FFN_CONV),
        "ffn_conv_b": small(ks[19], (DEPTH, 2 * D_FF), 0.02),
        "w_down": nrm(ks[20], (DEPTH, D_FF, D_MODEL), D_FF),
        "final_norm": gain(ks[21], (D_MODEL,)),
    }


def reference(x, attn_norm, w_in, qk_conv_w, qk_conv_b, i_bias, f_bias, mlstm_norm,
              cmp_pe_k, cmp_pe_v, cmp_w1_k, cmp_w2_k, cmp_w1_v, cmp_w2_v, nsa_norm,
              w_out, ffn_norm, w_up, ffn_conv_w, ffn_conv_b, w_down, final_norm):
    cos, sin = rope_tables(x.shape[1])
    for l in range(DEPTH):
        h = rmsnorm(x, attn_norm[l])
        x = x + hybrid_mixer(h, w_in[l], qk_conv_w[l], qk_conv_b[l], i_bias[l], f_bias[l],
                             mlstm_norm[l], cmp_pe_k[l], cmp_pe_v[l], cmp_w1_k[l], cmp_w2_k[l],
                             cmp_w1_v[l], cmp_w2_v[l], nsa_norm[l], w_out[l], cos, sin)
        h = rmsnorm(x, ffn_norm[l])
        x = x + conv_gated_mlp(h, w_up[l], ffn_conv_w[l], ffn_conv_b[l], w_down[l])
    return rmsnorm(x, final_norm)
```

```python
import numpy as np
import ml_dtypes
from contextlib import ExitStack
import concourse.bass as bass
import concourse.mybir as mybir
from concourse.bass_utils import run_bass_kernel_spmd

F32 = mybir.dt.float32
BF16 = mybir.dt.bfloat16
AF = mybir.ActivationFunctionType
ALU = mybir.AluOpType
AX = mybir.AxisListType

D = 2048
S_LEN = 2048
DEPTH = 2
N_IN = 6696
D_FF = 5632
NT = S_LEN // 128
EPS = 1e-6
SAME_ENGINE_SYNC = True

C_MQ, C_MK, C_MV, C_MO, C_MI, C_MF = 0, 1024, 2048, 3072, 4096, 4104
C_NQ, C_KC, C_VC, C_KS, C_VS, C_KW, C_VW, C_NG = 4112, 5136, 5392, 5648, 5904, 6160, 6416, 6672


class Buf:
    __slots__ = ("name", "writers", "readers", "sem", "semcnt", "excl")

    def __init__(self, name, excl=False):
        self.name = name
        self.excl = excl
        self.writers = {}
        self.readers = {}
        self.sem = None
        self.semcnt = 0


class Op:
    __slots__ = ("eng", "fn", "deps", "need_inc", "sem_val", "dma_buf", "dma_val", "dma_sem", "idx")

    def __init__(self, eng, fn):
        self.eng = eng
        self.fn = fn
        self.deps = []
        self.need_inc = False
        self.sem_val = 0
        self.dma_buf = None
        self.dma_val = 0


class Sched:
    ENGS = ("pe", "act", "dve", "pool", "sp")

    def __init__(self, nc, es):
        self.nc = nc
        self.es = es
        self.ops = {e: [] for e in self.ENGS}
        self.engsem = {e: es.enter_context(nc.semaphore("sem_" + e)) for e in ("pe", "act", "dve", "pool")}
        self.nops = 0
        self.sem_free = {"sw": [], "hw": []}
        self.phase_bufs = []
        self.nsem = 0

    @staticmethod
    def _key(tok):
        if tok[0] == "op":
            return tok[1].eng
        return ("dma", id(tok[1]))

    def add(self, eng, fn, reads=(), writes=(), dma_buf=None):
        op = Op(eng, fn)
        self.nops += 1
        if dma_buf is not None:
            typ = "sw" if eng == "pool" else "hw"
            if dma_buf.sem is None:
                dma_buf.sem = {}
            if typ not in dma_buf.sem:
                if self.sem_free[typ]:
                    dma_buf.sem[typ] = list(self.sem_free[typ].pop())
                else:
                    self.nsem += 1
                    dma_buf.sem[typ] = [self.es.enter_context(self.nc.semaphore("dsem_%d" % self.nsem)), 0]
                self.phase_bufs.append((dma_buf, typ))
            ent = dma_buf.sem[typ]
            ent[1] += 16
            op.dma_buf = dma_buf
            op.dma_sem = ent[0]
            op.dma_val = ent[1]
            tok = ("dma", ent[0], ent[1])
        else:
            tok = ("op", op)
        deps = []
        merge_flags = []
        xr = [b for b in reads if b.excl and b not in writes]
        if xr:
            reads = [b for b in reads if not b.excl]
            writes = list(writes) + xr
        for b in reads:
            deps.extend(b.writers.values())
        for b in writes:
            merge = (dma_buf is not None and not b.readers and b.writers
                     and all(w[0] == "dma" for w in b.writers.values()))
            merge_flags.append(merge)
            if not merge:
                deps.extend(b.writers.values())
                deps.extend(b.readers.values())
        op.deps = deps
        k = self._key(tok)
        for b in reads:
            b.readers[k] = tok
        for b, merge in zip(writes, merge_flags):
            if merge:
                b.writers[k] = tok
            else:
                b.writers = {k: tok}
                b.readers = {}
        self.ops[eng].append(op)
        return op

    def barrier(self, bufs):
        toks = []
        for b in bufs:
            toks.extend(b.writers.values())
            toks.extend(b.readers.values())
        for e in ("pe", "act", "dve", "pool"):
            if self.ops[e]:
                last = None
                for o in reversed(self.ops[e]):
                    if o.dma_buf is None and o.fn is not None:
                        last = o
                        break
                if last is not None:
                    toks.append(("op", last))
        for e in self.ENGS:
            op = Op(e, None)
            op.deps = list(toks)
            self.ops[e].append(op)

    def end_phase(self, bufs=()):
        self.barrier(list(bufs) + [b for b, _ in self.phase_bufs])
        for b, typ in self.phase_bufs:
            ent = b.sem.pop(typ)
            self.sem_free[typ].append((ent[0], ent[1]))
        self.phase_bufs = []

    def emit(self):
        nc = self.nc
        for e in self.ENGS:
            for op in self.ops[e]:
                for d in op.deps:
                    if d[0] == "op":
                        p = d[1]
                        if p.eng != e or (SAME_ENGINE_SYNC and e != "pe") or op.dma_buf is not None:
                            p.need_inc = True
        for e in self.ENGS:
            cnt = 0
            for op in self.ops[e]:
                if op.dma_buf is None and op.need_inc:
                    cnt += 1
                op.sem_val = cnt
        self.counts = {e: 0 for e in self.ENGS}

        def run(e, eng):
            waited = {}
            nins = 0
            for op in self.ops[e]:
                for d in op.deps:
                    if d[0] == "op":
                        p = d[1]
                        if p is op:
                            continue
                        if p.eng == e and not ((SAME_ENGINE_SYNC and e != "pe") or op.dma_buf is not None):
                            continue
                        sem, val = self.engsem[p.eng], p.sem_val
                    else:
                        sem, val = d[1], d[2]
                    key = id(sem)
                    if waited.get(key, 0) >= val:
                        continue
                    waited[key] = val
                    eng.wait_ge(sem, val)
                    nins += 1
                if op.fn is None:
                    continue
                ins = op.fn(eng)
                nins += 1
                if op.dma_buf is not None:
                    ins.then_inc(op.dma_sem, 16)
                elif op.need_inc:
                    ins.then_inc(self.engsem[e], 1)
            self.counts[e] = nins

        with nc.Block() as block:
            @block.tensor
            def _(eng):
                run("pe", eng)

            @block.scalar
            def _(eng):
                run("act", eng)

            @block.vector
            def _(eng):
                run("dve", eng)

            @block.gpsimd
            def _(eng):
                run("pool", eng)

            @block.sync
            def _(eng):
                run("sp", eng)


class Ctx:
    def __init__(self, nc, es):
        self.nc = nc
        self.es = es
        self.S = Sched(nc, es)
        self.uid = 0

    def sb(self, es, shape, dtype, name):
        self.uid += 1
        t = es.enter_context(self.nc.sbuf_tensor("%s_%d" % (name, self.uid), list(shape), dtype))
        return t, Buf(name)

    def dram(self, name, shape, dtype, kind="Internal"):
        t = self.nc.dram_tensor(name, list(shape), dtype, kind=kind)
        return t.ap(), Buf(name)


def dma(S, eng, out, in_, reads, writes, sbuf_buf):
    S.add(eng, lambda e: e.dma_start(out=out, in_=in_), reads=reads, writes=writes, dma_buf=sbuf_buf)


def mm(S, out, lhsT, rhs, start, stop, reads, writes):
    S.add("pe", lambda e: e.matmul(out, lhsT, rhs, start=start, stop=stop), reads, writes)


def tr(S, out, in_, ident, reads, writes):
    S.add("pe", lambda e: e.transpose(out=out, in_=in_, identity=ident), reads, writes)


def act(S, out, in_, func, reads, writes, **kw):
    S.add("act", lambda e: e.activation(out=out, in_=in_, func=func, **kw), reads, writes)


def tt(S, eng, out, in0, in1, op, reads, writes):
    S.add(eng, lambda e: e.tensor_tensor(out=out, in0=in0, in1=in1, op=op), reads, writes)


def ts(S, eng, out, in0, s1, s2, op0, op1, reads, writes):
    if s2 is None:
        S.add(eng, lambda e: e.tensor_scalar(out=out, in0=in0, scalar1=s1, scalar2=None, op0=op0), reads, writes)
    else:
        S.add(eng, lambda e: e.tensor_scalar(out=out, in0=in0, scalar1=s1, scalar2=s2, op0=op0, op1=op1), reads,
              writes)


def stt(S, out, in0, scalar, in1, op0, op1, reads, writes):
    S.add("dve", lambda e: e.scalar_tensor_tensor(out=out, in0=in0, scalar=scalar, in1=in1, op0=op0, op1=op1),
          reads, writes)


def cp(S, eng, out, in_, reads, writes):
    if eng == "act":
        S.add("act", lambda e: e.copy(out=out, in_=in_), reads, writes)
    else:
        S.add(eng, lambda e: e.tensor_copy(out=out, in_=in_), reads, writes)


class PanelBufs:
    def __init__(self, kg, bufs):
        self.kg = kg
        self.bufs = bufs

    def __call__(self, k):
        return self.bufs[k // self.kg]


class WStream:
    def __init__(self, cx, es, stage_elems, nstage, panel_elems, npanel):
        self.cx = cx
        self.stage_elems = stage_elems
        self.stages = [cx.sb(es, [128, stage_elems], F32, "wstg%d" % i) for i in range(nstage)]
        self.panels = [cx.sb(es, [128, panel_elems], BF16, "wpan%d" % i)[0] for i in range(npanel)]
        self.pbufs = [dict() for _ in range(npanel)]
        self.si = 0
        self.pi = 0

    def bufs(self):
        out = [b for _, b in self.stages]
        for d in self.pbufs:
            out.extend(d.values())
        return out

    def load(self, w_ap, w_buf, kch, ncols, q="sp"):
        S = self.cx.S
        pidx = self.pi % len(self.panels)
        pt = self.panels[pidx]
        self.pi += 1
        pv = pt[:, 0:kch * ncols].rearrange("p (k n) -> p k n", k=kch)
        kg = max(1, self.stage_elems // ncols)
        pend = None
        blist = []
        for gi, k0 in enumerate(range(0, kch, kg)):
            k1 = min(kch, k0 + kg)
            if gi not in self.pbufs[pidx]:
                self.pbufs[pidx][gi] = Buf("wpan%d_%d" % (pidx, gi))
            pb = self.pbufs[pidx][gi]
            blist.append(pb)
            st, sb_ = self.stages[self.si % len(self.stages)]
            self.si += 1
            sv = st[:, 0:(k1 - k0) * ncols].rearrange("p (k n) -> p k n", k=k1 - k0)
            dma(S, q, sv, w_ap[k0 * 128:k1 * 128, :].rearrange("(k p) n -> p k n", p=128), [w_buf], [sb_], sb_)
            if pend is not None:
                self._cast(*pend)
            pend = (pv, pb, sv, sb_, k0, k1, ncols)
        self._cast(*pend)
        return pv, PanelBufs(kg, blist)

    def _cast(self, pv, pb, sv, sb_, k0, k1, ncols):
        S = self.cx.S
        kc = max(1, 2048 // ncols)
        for a in range(k0, k1, kc):
            bnd = min(k1, a + kc)
            cp(S, "act", pv[:, a:bnd, :], sv[:, a - k0:bnd - k0, :], [sb_], [pb])


def phase_norm_T(cx, x_ap, x_buf, gain_ap, hT, hT_buf, G):
    S = cx.S
    with ExitStack() as es:
        gb, gb_b = cx.sb(es, [128, D], F32, "gb")
        xts = [cx.sb(es, [128, D], F32, "xt%d" % i) for i in range(2)]
        hns = [cx.sb(es, [128, D], BF16, "hn%d" % i) for i in range(2)]
        junk, junk_b = cx.sb(es, [128, D], BF16, "junk")
        st, st_b = cx.sb(es, [128, 4 * NT], F32, "stats")
        dma(S, "sp", gb[:], gain_ap.partition_broadcast(128), [], [gb_b], gb_b)
        for i in range(NT):
            xt, xt_b = xts[i % 2]
            hn, hn_b = hns[i % 2]
            dma(S, "sp", xt[:], x_ap[i * 128:(i + 1) * 128, :], [x_buf], [xt_b], xt_b)
            ss = st[:, 4 * i:4 * i + 1]
            lnv = st[:, 4 * i + 1:4 * i + 2]
            rstd = st[:, 4 * i + 2:4 * i + 3]
            act(S, junk[:], xt[:], AF.Square, [xt_b], [junk_b, st_b], accum_out=ss)
            ts(S, "dve", lnv, ss, 1.0 / D, EPS, ALU.mult, ALU.add, [st_b], [st_b])
            act(S, lnv, lnv, AF.Ln, [st_b], [st_b])
            act(S, rstd, lnv, AF.Exp, [st_b], [st_b], scale=-0.5)
            stt(S, hn[:], xt[:], rstd, gb[:], ALU.mult, ALU.mult, [xt_b, st_b, gb_b], [hn_b])
            for half in range(2):
                bi = (2 * i + half) % 8
                pb, pb_b = G.psum[bi], G.psum_b[bi]
                pT = pb[:].bitcast(BF16)
                for kk in range(8):
                    k = half * 8 + kk
                    tr(S, pT[:, kk * 128:(kk + 1) * 128], hn[:, k * 128:(k + 1) * 128], G.ident_bf[:],
                       [hn_b, G.cb], [pb_b])
                dst = hT[:, half * 8:half * 8 + 8, i * 128:(i + 1) * 128]
                src = pT.rearrange("p (k t) -> p k t", k=8)
                cp(S, "act" if half == 0 else "dve", dst, src, [pb_b], [hT_buf])
        S.end_phase([junk_b, st_b, hT_buf] + [b for _, b in hns])


ZCH = {"mq": 0, "mk": 8, "mo": 16, "nq": 24, "kc": 32, "ks": 34, "kw": 36, "vc": 38}
N_ZCH = 40


def phase_inproj(cx, l, hT, hT_b, G):
    S = cx.S
    W = G.w_in[l]
    Wb = G.wbuf
    with ExitStack() as es:
        ws = WStream(cx, es, 4096, 2, 8192, 2)
        cw, cw_b = cx.sb(es, [128, 16, 4], F32, "cw")
        cbias, cbias_b = cx.sb(es, [128, 16], F32, "cbias")
        gbias, gbias_b = cx.sb(es, [16, 1], F32, "gbias")
        ropeC, ropeC_b = cx.sb(es, [32, S_LEN], F32, "ropeC")
        ropeS, ropeS_b = cx.sb(es, [32, S_LEN], F32, "ropeS")
        dma(S, "sp", cw[:], G.qk_cw[l], [], [cw_b], cw_b)
        dma(S, "sp", cbias[:], G.qk_cb[l], [], [cbias_b], cbias_b)
        dma(S, "sp", gbias[:], G.gbias[l], [], [gbias_b], gbias_b)
        dma(S, "sp", ropeC[:], G.ropeC_in, [], [ropeC_b], ropeC_b)
        dma(S, "sp", ropeS[:], G.ropeS_in, [], [ropeS_b], ropeS_b)
        stgs = [cx.sb(es, [128, S_LEN + 4], F32, "cstg%d" % i) for i in range(2)]
        ys = [cx.sb(es, [128, S_LEN], F32, "cy%d" % i) for i in range(1)]
        zos = [cx.sb(es, [128, S_LEN], BF16, "zo%d" % i) for i in range(3)]
        t1s = [cx.sb(es, [32, 512], F32, "rt1_%d" % i) for i in range(2)]
        t2s = [cx.sb(es, [32, 512], F32, "rt2_%d" % i) for i in range(2)]
        gst, gst_b = cx.sb(es, [32, S_LEN], F32, "gst")
        tmo = [cx.sb(es, [128, 512], BF16, "tmo%d" % i) for i in range(3)]
        for st_, sb_ in stgs:
            S.add("pool", lambda e, st_=st_: e.memset(st_[:, 0:4], 0.0), [], [sb_])
        state = {"grp": 0, "zo": 0, "cs": 0, "r": 0, "tm": 0, "tb": 0}

        fm_panels = []
        for p in range(4):
            fm_panels.append(("qk", 512 * p, 512, 4 * p))
        for p in range(2):
            fm_panels.append(("plain", C_MO + 512 * p, 512, ZCH["mo"] + 4 * p))
        fm_panels.append(("gif", C_MI, 16, None))
        for p in range(2):
            fm_panels.append(("rope", C_NQ + 512 * p, 512, ZCH["nq"] + 4 * p))
        fm_panels.append(("rope", C_KC, 256, ZCH["kc"]))
        fm_panels.append(("plain", C_VC, 256, ZCH["vc"]))
        fm_panels.append(("rope", C_KS, 256, ZCH["ks"]))
        fm_panels.append(("rope", C_KW, 256, ZCH["kw"]))
        fm_panels.append(("gng", C_NG, 24, None))
        tm_panels = [(C_MV, 512, 0), (C_MV + 512, 512, 512), (C_VS, 256, 1024), (C_VW, 256, 1280)]
        panels = [("fm",) + p for p in fm_panels] + [("tm",) + p for p in tm_panels]
        import os as _os
        if _os.environ.get("K_DBG_KINDS"):
            kinds = _os.environ["K_DBG_KINDS"].split(",")
            panels = [p for p in panels if (p[1] if p[0] == "fm" else "tm") in kinds]

        def load(p):
            if p[0] == "fm":
                _, kind, c0, nc_, ch = p
            else:
                _, c0, nc_, zc = p
            return ws.load(W[:, c0:c0 + nc_], Wb, 16, nc_)

        def _inproj_post(kind, ch0, m, banks):
            if kind == "qk":
                c = ch0 + m
                st_, sb_ = stgs[state["cs"] % 2]
                y, y_b = ys[0]
                state["cs"] += 1
                zo, zo_b = zos[state["zo"] % 3]
                state["zo"] += 1
                for n in range(4):
                    b = banks[n]
                    cp(S, "act", st_[:, 3 + 512 * n:3 + 512 * n + 512], G.psum[b][:], [G.psum_b[b]], [sb_])
                ts(S, "dve", y[:], st_[:, 3:3 + S_LEN], cw[:, c, 3:4], cbias[:, c:c + 1], ALU.mult, ALU.add,
                   [sb_, cw_b, cbias_b], [y_b])
                for j in range(3):
                    stt(S, y[:], st_[:, j:j + S_LEN], cw[:, c, j:j + 1], y[:], ALU.mult, ALU.add,
                        [sb_, cw_b, y_b], [y_b])
                act(S, zo[:], y[:], AF.Silu, [y_b], [zo_b])
                dma(S, "pool", G.zfm[c], zo[:], [zo_b], [G.zfm_b], zo_b)
            elif kind == "plain":
                zo, zo_b = zos[state["zo"] % 3]
                state["zo"] += 1
                for n in range(4):
                    b = banks[n]
                    cp(S, "act" if n % 2 == 0 else "dve", zo[:, 512 * n:512 * n + 512], G.psum[b][:],
                       [G.psum_b[b]], [zo_b])
                dma(S, "pool", G.zfm[ch0 + m], zo[:], [zo_b], [G.zfm_b], zo_b)
            elif kind == "rope":
                zo, zo_b = zos[state["zo"] % 3]
                state["zo"] += 1
                for n in range(4):
                    b = banks[n]
                    sl = slice(512 * n, 512 * n + 512)
                    t1, t1_b = t1s[state["r"] % 2]
                    t2, t2_b = t2s[state["r"] % 2]
                    state["r"] += 1
                    cp(S, "act", zo[:, sl], G.psum[b][:], [G.psum_b[b]], [zo_b])
                    tt(S, "dve", t1[:], G.psum[b][0:32, :], ropeC[:, sl], ALU.mult, [G.psum_b[b], ropeC_b],
                       [t1_b])
                    mm(S, G.psum[b][:], G.rot32[:], zo[:, sl], True, True, [zo_b, G.cb],
                       [G.psum_b[b]])
                    tt(S, "dve", t2[:], G.psum[b][0:32, :], ropeS[:, sl], ALU.mult, [G.psum_b[b], ropeS_b],
                       [t2_b])
                    tt(S, "dve", zo[0:32, sl], t1[:], t2[:], ALU.add, [t1_b, t2_b], [zo_b])
                dma(S, "pool", G.zfm[ch0 + m], zo[:], [zo_b], [G.zfm_b], zo_b)
            elif kind == "gif":
                for n in range(4):
                    b = banks[n]
                    ts(S, "dve", gst[0:16, 512 * n:512 * n + 512], G.psum[b][0:16, :], gbias[:, 0:1], None,
                       ALU.add, None, [G.psum_b[b], gbias_b], [gst_b])
                dma(S, "pool", G.gif, gst[0:16, :], [gst_b], [G.gif_b], gst_b)
            elif kind == "gng":
                for n in range(4):
                    b = banks[n]
                    act(S, gst[0:24, 512 * n:512 * n + 512], G.psum[b][0:24, :], AF.Sigmoid, [G.psum_b[b]],
                        [gst_b])
                dma(S, "pool", G.gng, gst[0:24, :], [gst_b], [G.gng_b], gst_b)

        nxt = load(panels[0])
        for pi, p in enumerate(panels):
            wv, wv_b = nxt
            if pi + 1 < len(panels):
                nxt = load(panels[pi + 1])
            if p[0] == "fm":
                _, kind, c0, nc_, ch0 = p
                nchunk = (nc_ + 127) // 128
                for m in range(nchunk):
                    mw = min(128, nc_ - 128 * m)
                    grp = state["grp"] % 2
                    state["grp"] += 1
                    banks = [4 * grp + n for n in range(4)]
                    for k in range(16):
                        for n in range(4):
                            b = banks[n]
                            mm(S, G.psum[b][0:mw, :], wv[:, k, 128 * m:128 * m + mw], hT[:, k, 512 * n:512 * n + 512],
                               k == 0, k == 15, [wv_b(k), hT_b], [G.psum_b[b]])
                    post_prev = state.get("post")

                    def post(kind=kind, ch0=ch0, m=m, banks=banks):
                        _inproj_post(kind, ch0, m, banks)

                    state["post"] = post
                    if post_prev is not None:
                        post_prev()
            else:
                if state.get("post") is not None:
                    state["post"]()
                    state["post"] = None
                _, c0, nc_, zc = p
                for i in range(NT):
                    b = state["tb"] % 8
                    state["tb"] += 1
                    for k in range(16):
                        mm(S, G.psum[b][:, 0:nc_], hT[:, k, 128 * i:128 * i + 128], wv[:, k, :], k == 0, k == 15,
                           [wv_b(k), hT_b], [G.psum_b[b]])
                    to, to_b = tmo[state["tm"] % 3]
                    state["tm"] += 1
                    cp(S, "act" if i % 2 == 0 else "dve", to[:, 0:nc_], G.psum[b][:, 0:nc_], [G.psum_b[b]], [to_b])
                    dma(S, "pool", G.ztm[128 * i:128 * i + 128, zc:zc + nc_], to[:, 0:nc_], [to_b], [G.ztm_b], to_b)
        if state.get("post") is not None:
            state["post"]()
            state["post"] = None
        allb = ws.bufs() + [cw_b, cbias_b, gbias_b, ropeC_b, ropeS_b, gst_b, G.zfm_b, G.ztm_b, G.gif_b, G.gng_b]
        allb += [b for _, b in stgs + ys + zos + t1s + t2s + tmo]
        S.end_phase(allb + G.psum_b)
DSCALE = 128.0 ** -0.5


def run_pipeline(items, la, d3=2):
    n = len(items)
    for idx in range(n + la + d3):
        k = idx - la - d3
        if 0 <= k < n and items[k][2] is not None:
            items[k][2]()
        if idx < n:
            items[idx][0]()
        k = idx - la
        if 0 <= k < n:
            items[k][1]()


def phase_mlstm(cx, l, G):
    S = cx.S
    with ExitStack() as es:
        def t8(name):
            return cx.sb(es, [8, S_LEN], F32, name)
        gi, gi_b = t8("gi")
        gf, gf_b = t8("gf")
        ones8, ones8_b = t8("ones8")
        zeros8, zeros8_b = t8("zeros8")
        nF, nF_b = t8("nF")
        Mx, Mx_b = t8("Mx")
        u, u_b = cx.sb(es, [128, S_LEN], F32, "u")
        negM, negM_b = cx.sb(es, [128, S_LEN], F32, "negM")
        negm, negm_b = cx.sb(es, [128, S_LEN], F32, "negm")
        for t_, b_ in ((u, u_b), (negM, negM_b), (negm, negm_b)):
            S.add("pool", lambda e, t_=t_: e.memset(t_[:], 0.0), [], [b_])
        sel8, sel8_b = cx.sb(es, [128, 1024], F32, "sel8")
        mnorm, mnorm_b = cx.sb(es, [128, 8], F32, "mnorm")
        uT, uT_b = cx.sb(es, [128, 128], F32, "uT")
        dma(S, "sp", gi[:], G.gif[0:8, :], [G.gif_b], [gi_b], gi_b)
        dma(S, "sp", gf[:], G.gif[8:16, :], [G.gif_b], [gf_b], gf_b)
        dma(S, "sp", sel8[:], G.cin["sel8"], [], [sel8_b], sel8_b)
        dma(S, "sp", mnorm[:], G.mnorm[l], [], [mnorm_b], mnorm_b)
        S.add("pool", lambda e: e.memset(ones8[:], 1.0), [], [ones8_b])
        S.add("pool", lambda e: e.memset(zeros8[:], 0.0), [], [zeros8_b])
        act(S, gf[:], gf[:], AF.Exp, [gf_b], [gf_b], scale=-1.0)
        act(S, gf[:], gf[:], AF.Ln, [gf_b], [gf_b], bias=1.0)
        S.add("dve", lambda e: e.tensor_tensor_scan(out=nF[:], data0=ones8[:], data1=gf[:], initial=0.0,
                                                    op0=ALU.mult, op1=ALU.add), [ones8_b, gf_b], [nF_b])
        tt(S, "dve", u[0:8, :], gi[:], nF[:], ALU.add, [gi_b, nF_b], [u_b])
        S.add("dve", lambda e: e.tensor_tensor_scan(out=Mx[:], data0=u[0:8, :], data1=zeros8[:], initial=0.0,
                                                    op0=ALU.max, op1=ALU.max), [u_b, zeros8_b], [Mx_b])
        ts(S, "dve", negM[0:8, :], Mx[:], -1.0, None, ALU.mult, None, [Mx_b], [negM_b])
        tt(S, "dve", negm[0:8, :], nF[:], Mx[:], ALU.subtract, [nF_b, Mx_b], [negm_b])
        for i in range(NT):
            tr(S, G.psum[i // 4][:, 128 * (i % 4):128 * (i % 4) + 128], u[:, 128 * i:128 * i + 128], G.ident_f[:],
               [u_b, G.cb], [G.psum_b[i // 4]])
        for i in range(NT):
            cp(S, "dve", uT[:, 8 * i:8 * i + 8], G.psum[i // 4][:, 128 * (i % 4):128 * (i % 4) + 8],
               [G.psum_b[i // 4]], [uT_b])

        qs = [cx.sb(es, [128, S_LEN], BF16, "mq%d" % i) for i in range(2)]
        ks = [cx.sb(es, [128, S_LEN], BF16, "mk%d" % i) for i in range(2)]
        os_ = [cx.sb(es, [128, S_LEN], BF16, "mo%d" % i) for i in range(2)]
        vs = [cx.sb(es, [128, NT, 128], BF16, "mv%d" % i) for i in range(2)]
        outs = [cx.sb(es, [128, S_LEN], BF16, "mout%d" % i) for i in range(2)]
        negMb, negMb_b = cx.sb(es, [128, S_LEN], F32, "negMb")
        negmb, negmb_b = cx.sb(es, [128, S_LEN], F32, "negmb")
        wTs = [cx.sb(es, [128, 512], F32, "wT%d" % i) for i in range(4)]
        pTs = [cx.sb(es, [128, 512], BF16, "pT%d" % i) for i in range(3)]
        em, em_b = cx.sb(es, [128, 512], F32, "em")
        dn, dn_b = cx.sb(es, [128, 512], F32, "dn")
        hraw, hraw_b = cx.sb(es, [128, 512], F32, "hraw")
        sq, sq_b = cx.sb(es, [128, 512], BF16, "sq")
        rstd, rstd_b = cx.sb(es, [128, 512], F32, "rstd")
        og, og_b = cx.sb(es, [128, 512], F32, "og")
        rr = {"w": 0, "p": 0, "s": 0}

        def load_head(hh):
            q, q_b = qs[hh % 2]
            k, k_b = ks[hh % 2]
            o, o_b = os_[hh % 2]
            v, v_b = vs[hh % 2]
            dma(S, "sp", q[:], G.zfm[ZCH["mq"] + hh], [G.zfm_b], [q_b], q_b)
            dma(S, "sp", k[:], G.zfm[ZCH["mk"] + hh], [G.zfm_b], [k_b], k_b)
            dma(S, "sp", o[:], G.zfm[ZCH["mo"] + hh], [G.zfm_b], [o_b], o_b)
            dma(S, "sp", v[:], G.ztm[:, 128 * hh:128 * hh + 128].rearrange("(t p) d -> p t d", p=128), [G.ztm_b],
                [v_b], v_b)

        negMbs = [(negMb, negMb_b), cx.sb(es, [128, S_LEN], F32, "negMb1")]
        negmbs = [(negmb, negmb_b), cx.sb(es, [128, S_LEN], F32, "negmb1")]
        items = []
        load_head(0)
        for hh in range(8):
            q, q_b = qs[hh % 2]
            k, k_b = ks[hh % 2]
            o, o_b = os_[hh % 2]
            v, v_b = vs[hh % 2]
            mo, mo_b = outs[hh % 2]
            nMb, nMb_b = negMbs[hh % 2]
            nmb, nmb_b = negmbs[hh % 2]

            def prologue(hh=hh, nMb=nMb, nMb_b=nMb_b, nmb=nmb, nmb_b=nmb_b):
                for n in range(4):
                    sl = slice(512 * n, 512 * n + 512)
                    pb = 2 * (n % 2)
                    mm(S, G.psum[pb][:], sel8[:, 128 * hh:128 * hh + 128], negM[:, sl], True, True,
                       [sel8_b, negM_b], [G.psum_b[pb]])
                    cp(S, "act", nMb[:, sl], G.psum[pb][:], [G.psum_b[pb]], [nMb_b])
                for n in range(4):
                    sl = slice(512 * n, 512 * n + 512)
                    pb = 2 * (n % 2)
                    mm(S, G.psum[pb][:], sel8[:, 128 * hh:128 * hh + 128], negm[:, sl], True, True,
                       [sel8_b, negm_b], [G.psum_b[pb]])
                    cp(S, "dve", nmb[:, sl], G.psum[pb][:], [G.psum_b[pb]], [nmb_b])

            for I in range(4):
                nJ = 4 * I + 4
                for J in range(nJ):
                    st = {}

                    def stage1(hh=hh, I=I, J=J, st=st, q=q, q_b=q_b, k=k, k_b=k_b, nMb=nMb, nMb_b=nMb_b,
                               prologue=prologue):
                        if I == 0 and J == 0:
                            prologue()
                        if I == 1 and J == 3 and hh + 1 < 8:
                            load_head(hh + 1)
                        col0 = max(0, 128 * J - 512 * I)
                        ncol = 512 - col0
                        q0 = 512 * I + col0
                        st["sb"] = 4 + rr["s"] % 4
                        rr["s"] += 1
                        st["wT"] = wTs[rr["w"] % 4]
                        rr["w"] += 1
                        sb_i = st["sb"]
                        wT, wT_b = st["wT"]
                        mm(S, G.psum[sb_i][:, 0:ncol], k[:, 128 * J:128 * J + 128], q[:, q0:q0 + ncol], True, True,
                           [k_b, q_b], [G.psum_b[sb_i]])
                        act(S, wT[:, 0:ncol], nMb[:, q0:q0 + ncol], AF.Exp, [nMb_b, uT_b], [wT_b],
                            bias=uT[:, 8 * J + hh:8 * J + hh + 1])

                    def stage2(hh=hh, I=I, J=J, nJ=nJ, st=st, v=v, v_b=v_b, o=o, o_b=o_b, mo=mo, mo_b=mo_b,
                               nmb=nmb, nmb_b=nmb_b):
                        nb, db = I % 2, 2 + (I % 2)
                        sl = slice(512 * I, 512 * I + 512)
                        col0 = max(0, 128 * J - 512 * I)
                        ncol = 512 - col0
                        sb_i = st["sb"]
                        wT, wT_b = st["wT"]
                        pT, pT_b = pTs[rr["p"] % 3]
                        rr["p"] += 1
                        stt(S, pT[:, 0:ncol], G.psum[sb_i][:, 0:ncol], DSCALE, wT[:, 0:ncol], ALU.mult, ALU.mult,
                            [G.psum_b[sb_i], wT_b], [pT_b])
                        if J >= 4 * I:
                            tt(S, "dve", pT[:, 0:128], pT[:, 0:128], G.tri[:], ALU.mult, [pT_b, G.cb], [pT_b])
                        mm(S, G.psum[nb][:, col0:512], v[:, J, :], pT[:, 0:ncol], J == 0, J == nJ - 1, [v_b, pT_b],
                           [G.psum_b[nb]])
                        mm(S, G.psum[db][:, col0:512], G.ones_bf[:], pT[:, 0:ncol], J == 0, J == nJ - 1, [G.cb, pT_b],
                           [G.psum_b[db]])

                    def stage3(hh=hh, I=I, o=o, o_b=o_b, mo=mo, mo_b=mo_b, nmb=nmb, nmb_b=nmb_b):
                        nb, db = I % 2, 2 + (I % 2)
                        sl = slice(512 * I, 512 * I + 512)
                        act(S, em[:], nmb[:, sl], AF.Exp, [nmb_b], [em_b])
                        act(S, dn[:], G.psum[db][:], AF.Abs, [G.psum_b[db]], [dn_b])
                        tt(S, "dve", dn[:], dn[:], em[:], ALU.max, [dn_b, em_b], [dn_b])
                        act(S, dn[:], dn[:], AF.Ln, [dn_b], [dn_b])
                        act(S, dn[:], dn[:], AF.Exp, [dn_b], [dn_b], scale=-1.0)
                        tt(S, "dve", hraw[:], G.psum[nb][:], dn[:], ALU.mult, [G.psum_b[nb], dn_b], [hraw_b])
                        act(S, sq[:], hraw[:], AF.Square, [hraw_b], [sq_b])
                        mm(S, G.psum[db][:], G.ones_bf[:], sq[:], True, True, [G.cb, sq_b], [G.psum_b[db]])
                        act(S, rstd[:], G.psum[db][:], AF.Ln, [G.psum_b[db]], [rstd_b], scale=1.0 / 128, bias=EPS)
                        act(S, rstd[:], rstd[:], AF.Exp, [rstd_b], [rstd_b], scale=-0.5)
                        act(S, og[:], o[:, sl], AF.Sigmoid, [o_b], [og_b])
                        tt(S, "dve", hraw[:], hraw[:], rstd[:], ALU.mult, [hraw_b, rstd_b], [hraw_b])
                        stt(S, mo[:, sl], hraw[:], mnorm[:, hh:hh + 1], og[:], ALU.mult, ALU.mult,
                            [hraw_b, mnorm_b, og_b], [mo_b])
                        if I == 3:
                            dma(S, "pool", G.mix[hh], mo[:], [mo_b], [G.mix_b], mo_b)

                    items.append((stage1, stage2, stage3 if J == nJ - 1 else None))
        run_pipeline(items, 3)
        S.end_phase()


def phase_nsa(cx, l, G):
    S = cx.S
    with ExitStack() as es:
        kcmpT = [cx.sb(es, [128, 128], BF16, "kcmpT%d" % i) for i in range(2)]
        vcmp = [cx.sb(es, [128, 128], BF16, "vcmp%d" % i) for i in range(2)]
        with ExitStack() as es2:
            ws = WStream(cx, es2, 8192, 2, 8192, 2)
            pes = [cx.sb(es2, [128, 32], F32, "pe%d" % i) for i in range(2)]
            srcs = [cx.sb(es2, [128, S_LEN], BF16, "csrc%d" % i) for i in range(2)]
            stats = [cx.sb(es2, [128, 32, 128], BF16, "cstat%d" % i) for i in range(2)]
            xs, xs_b = cx.sb(es2, [128, 128], F32, "gx")
            x2, x2_b = cx.sb(es2, [128, 128], F32, "gx2")
            sg, sg_b = cx.sb(es2, [128, 128], F32, "gsg")
            hid = [cx.sb(es2, [128, 128], BF16, "hid%d" % i) for i in range(2)]
            dma(S, "sp", pes[0][0][:], G.pe_k[l], [], [pes[0][1]], pes[0][1])
            dma(S, "sp", pes[1][0][:], G.pe_v[l], [], [pes[1][1]], pes[1][1])
            cnt = 0
            for kind in (0, 1):
                w1 = (G.cmp_w1_k if kind == 0 else G.cmp_w1_v)[l]
                w2 = (G.cmp_w2_k if kind == 0 else G.cmp_w2_v)[l]
                W1, W1_b = ws.load(w1, G.wbuf, 32, 256)
                W2, W2_b = ws.load(w2, G.wbuf, 2, 128)
                pe, pe_b = pes[kind]
                for h in range(2):
                    src, src_b = srcs[cnt % 2]
                    stat, stat_b = stats[cnt % 2]
                    cnt += 1
                    dma(S, "sp", src[:], G.zfm[(ZCH["kc"] if kind == 0 else ZCH["vc"]) + h], [G.zfm_b], [src_b],
                        src_b)
                    src3 = src[:].rearrange("p (b r) -> p b r", r=16)
                    for lp in range(32):
                        a, r_ = lp // 16, lp % 16
                        ts(S, "dve", stat[:, lp, 0:127], src3[:, a:a + 127, r_], pe[:, lp:lp + 1], None, ALU.add, None,
                           [src_b, pe_b], [stat_b])
                    for j in range(2):
                        b = j
                        for lp in range(32):
                            mm(S, G.psum[b][:, 0:127], W1[:, lp, 128 * j:128 * j + 128], stat[:, lp, 0:127], lp == 0,
                               lp == 31, [W1_b(lp), stat_b], [G.psum_b[b]])
                        hj, hj_b = hid[j]
                        cp(S, "act", xs[:, 0:127], G.psum[b][:, 0:127], [G.psum_b[b]], [xs_b])
                        tt(S, "dve", x2[:, 0:127], xs[:, 0:127], xs[:, 0:127], ALU.mult, [xs_b], [x2_b])
                        ts(S, "dve", x2[:, 0:127], x2[:, 0:127], 0.044715, 1.0, ALU.mult, ALU.add, [x2_b], [x2_b])
                        tt(S, "dve", x2[:, 0:127], x2[:, 0:127], xs[:, 0:127], ALU.mult, [x2_b, xs_b], [x2_b])
                        act(S, sg[:, 0:127], x2[:, 0:127], AF.Sigmoid, [x2_b], [sg_b], scale=2.0 * 0.7978845608028654)
                        tt(S, "dve", hj[:, 0:127], xs[:, 0:127], sg[:, 0:127], ALU.mult, [xs_b, sg_b], [hj_b])
                    if kind == 0:
                        for j in range(2):
                            mm(S, G.psum[2][:, 0:127], W2[:, j, :], hid[j][0][:, 0:127], j == 0, j == 1,
                               [W2_b(j), hid[j][1]], [G.psum_b[2]])
                        cp(S, "act", kcmpT[h][0][:, 0:127], G.psum[2][:, 0:127], [G.psum_b[2]], [kcmpT[h][1]])
                    else:
                        for j in range(2):
                            mm(S, G.psum[2][0:127, 0:128], hid[j][0][:, 0:127], W2[:, j, :], j == 0, j == 1,
                               [W2_b(j), hid[j][1]], [G.psum_b[2]])
                        cp(S, "act", vcmp[h][0][0:127, :], G.psum[2][0:127, 0:128], [G.psum_b[2]], [vcmp[h][1]])
            S.end_phase()

        with ExitStack() as es3:
            def cst(name, shape, dt_):
                t, b = cx.sb(es3, shape, dt_, name)
                dma(S, "sp", t[:], G.cin[name], [], [b], b)
                return t, b
            cmask, cmask_b = cst("cmask", [128, S_LEN], BF16)
            overlap, overlap_b = cst("overlap", [128, 32], BF16)
            impA, impA_b = cst("impA", [128, 512], F32)
            impB, impB_b = cst("impB", [128, 512], F32)
            esel, esel_b = cst("esel", [128, 2048], BF16)
            trilo, trilo_b = cst("trilo", [128, 128], BF16)
            qT4, qT4_b = cx.sb(es3, [128, 4, S_LEN], BF16, "qT4")
            acc, acc_b = cx.sb(es3, [128, 4, S_LEN], F32, "acc")
            mk_all, mk_all_b = cx.sb(es3, [128, 40, 512], BF16, "mk_all")
            ksT, ksT_b = cx.sb(es3, [128, S_LEN], BF16, "ksT")
            kwT, kwT_b = cx.sb(es3, [128, S_LEN], BF16, "kwT")
            vs_sb, vs_b = cx.sb(es3, [128, NT, 128], BF16, "vs_sb")
            vw_sb, vw_b = cx.sb(es3, [128, NT, 128], BF16, "vw_sb")
            pnsum, pnsum_b = cx.sb(es3, [128, S_LEN], F32, "pnsum")
            pnbf, pnbf_b = cx.sb(es3, [128, S_LEN], BF16, "pnbf")
            imp2, imp2_b = cx.sb(es3, [128, 512], F32, "imp2")
            impw, impw_b = cx.sb(es3, [128, 512], F32, "impw")
            m8, m8_b = cx.sb(es3, [128, 128], F32, "m8")
            m8b, m8b_b = cx.sb(es3, [128, 128], F32, "m8b")
            sel, sel_b = cx.sb(es3, [128, 512], BF16, "sel")
            selT, selT_b = cx.sb(es3, [128, S_LEN], BF16, "selT")
            S.add("pool", lambda e: e.memset(selT[:], 0.0), [], [selT_b])
            gts = [cx.sb(es3, [128, 512], F32, "gate%d" % i) for i in range(5)]
            es_ = [cx.sb(es3, [128, 512], F32, "ef%d" % i) for i in range(3)]
            ebs = [cx.sb(es3, [128, 512], BF16, "eb%d" % i) for i in range(3)]
            pTs = [cx.sb(es3, [128, 512], BF16, "npT%d" % i) for i in range(5)]
            rdens = [cx.sb(es3, [128, 512], F32, "rden%d" % i) for i in range(3)]
            t1s = [cx.sb(es3, [128, 512], F32, "nt1_%d" % i) for i in range(3)]
            t2s = [cx.sb(es3, [128, 512], F32, "nt2_%d" % i) for i in range(3)]
            rr = {"g": 0, "e": 0, "eb": 0, "p": 0, "r": 0, "t": 0, "s": 0, "a": 0}

            def gate_tile(idx, sl):
                gt, gt_b = gts[rr["g"] % 5]
                rr["g"] += 1
                dma(S, "sp", gt[:], G.gng[idx, sl].partition_broadcast(128), [G.gng_b], [gt_b], gt_b)
                return gt, gt_b

            for h in range(2):
                for g in range(4):
                    dma(S, "sp", qT4[:, g, :], G.zfm[ZCH["nq"] + 4 * h + g], [G.zfm_b], [qT4_b], qT4_b)
                dma(S, "sp", ksT[:], G.zfm[ZCH["ks"] + h], [G.zfm_b], [ksT_b], ksT_b)
                dma(S, "sp", kwT[:], G.zfm[ZCH["kw"] + h], [G.zfm_b], [kwT_b], kwT_b)
                dma(S, "sp", vs_sb[:], G.ztm[:, 1024 + 128 * h:1024 + 128 * h + 128].rearrange("(t p) d -> p t d", p=128),
                    [G.ztm_b], [vs_b], vs_b)
                dma(S, "sp", vw_sb[:], G.ztm[:, 1280 + 128 * h:1280 + 128 * h + 128].rearrange("(t p) d -> p t d", p=128),
                    [G.ztm_b], [vw_b], vw_b)
                kc_t, kc_b = kcmpT[h]
                vc_t, vc_b = vcmp[h]
                items = []
                for g in range(4):
                    hd = 4 * h + g
                    for I in range(4):
                        st = {}

                        def stage1(g=g, hd=hd, I=I, st=st):
                            sl = slice(512 * I, 512 * I + 512)
                            st["gt"] = gate_tile(hd * 3 + 0, sl)
                            st["sb"] = 4 + rr["s"] % 4
                            rr["s"] += 1
                            sb_i = st["sb"]
                            mm(S, G.psum[sb_i][0:127, :], kc_t[:, 0:127], qT4[:, g, sl], True, True, [kc_b, qT4_b],
                               [G.psum_b[sb_i]])

                        def stage2(g=g, I=I, st=st):
                            sl = slice(512 * I, 512 * I + 512)
                            sb_i = st["sb"]
                            st["ob"], st["db"] = rr["a"] % 2, 2 + rr["a"] % 2
                            rr["a"] += 1
                            ob, db = st["ob"], st["db"]
                            ef, ef_b = es_[rr["e"] % 3]
                            rr["e"] += 1
                            st["pT"] = pTs[rr["p"] % 5]
                            rr["p"] += 1
                            pT, pT_b = st["pT"]
                            act(S, ef[0:127, :], G.psum[sb_i][0:127, :], AF.Exp, [G.psum_b[sb_i]], [ef_b], scale=DSCALE)
                            tt(S, "dve", pT[0:127, :], ef[0:127, :], cmask[0:127, sl], ALU.mult, [ef_b, cmask_b], [pT_b])
                            mm(S, G.psum[ob][:], vc_t[0:127, :], pT[0:127, :], True, True, [vc_b, pT_b], [G.psum_b[ob]])
                            mm(S, G.psum[db][:], G.ones_bf[0:127, :], pT[0:127, :], True, True, [G.cb, pT_b],
                               [G.psum_b[db]])

                        def stage3(g=g, I=I, st=st):
                            sl = slice(512 * I, 512 * I + 512)
                            ob, db = st["ob"], st["db"]
                            pT, pT_b = st["pT"]
                            gt, gt_b = st["gt"]
                            rden, rden_b = rdens[rr["r"] % 3]
                            rr["r"] += 1
                            t1, t1_b = t1s[rr["t"] % 3]
                            t2, t2_b = t2s[rr["t"] % 3]
                            rr["t"] += 1
                            ts(S, "dve", rden[:], G.psum[db][:], 1e-30, None, ALU.max, None, [G.psum_b[db]], [rden_b])
                            act(S, rden[:], rden[:], AF.Ln, [rden_b], [rden_b])
                            act(S, rden[:], rden[:], AF.Exp, [rden_b], [rden_b], scale=-1.0)
                            tt(S, "dve", t1[:], G.psum[ob][:], rden[:], ALU.mult, [G.psum_b[ob], rden_b], [t1_b])
                            tt(S, "dve", acc[:, g, sl], t1[:], gt[:], ALU.mult, [t1_b, gt_b], [acc_b])
                            if g == 0:
                                tt(S, "dve", pnsum[0:127, sl], pT[0:127, :], rden[0:127, :], ALU.mult, [pT_b, rden_b],
                                   [pnsum_b])
                            else:
                                tt(S, "dve", t2[0:127, :], pT[0:127, :], rden[0:127, :], ALU.mult, [pT_b, rden_b], [t2_b])
                                tt(S, "dve", pnsum[0:127, sl], pnsum[0:127, sl], t2[0:127, :], ALU.add,
                                   [pnsum_b, t2_b], [pnsum_b])

                        items.append((stage1, stage2, stage3))
                run_pipeline(items, 2, 1)
                cp(S, "act", pnbf[0:127, :], pnsum[0:127, :], [pnsum_b], [pnbf_b])
                for i in range(NT):
                    mm(S, G.psum[4][:, 32 * i:32 * i + 32], pnbf[0:127, 128 * i:128 * i + 128], overlap[0:127, :], True,
                       True, [pnbf_b, overlap_b], [G.psum_b[4]])
                tt(S, "dve", imp2[:], G.psum[4][:], impA[:], ALU.mult, [G.psum_b[4], impA_b], [imp2_b])
                tt(S, "dve", imp2[:], imp2[:], impB[:], ALU.add, [imp2_b, impB_b], [imp2_b])
                for i in range(NT):
                    s32 = slice(32 * i, 32 * i + 32)
                    s8 = slice(8 * i, 8 * i + 8)
                    S.add("dve", lambda e, s32=s32, s8=s8: e.max(out=m8[:, s8], in_=imp2[:, s32]), [imp2_b], [m8_b])
                    S.add("dve", lambda e, s32=s32, s8=s8: e.match_replace(out=impw[:, s32], in_to_replace=m8[:, s8],
                                                                        in_values=imp2[:, s32], imm_value=-1e30),
                          [imp2_b, m8_b], [impw_b])
                    S.add("dve", lambda e, s32=s32, s8=s8: e.max(out=m8b[:, s8], in_=impw[:, s32]), [impw_b], [m8b_b])
                    ts(S, "dve", sel[:, s32], imp2[:, s32], m8b[:, 8 * i + 7:8 * i + 8], None, ALU.is_ge, None,
                       [imp2_b, m8b_b], [sel_b])
                for half in range(2):
                    pb = G.psum[5 + half]
                    pTv = pb[:].bitcast(BF16)
                    for ii in range(8):
                        i = 8 * half + ii
                        tr(S, pTv[0:32, 128 * ii:128 * ii + 128], sel[:, 32 * i:32 * i + 32], G.ident_bf[:],
                           [sel_b, G.cb], [G.psum_b[5 + half]])
                    cp(S, "act", selT[0:32, 1024 * half:1024 * half + 1024], pTv[0:32, :], [G.psum_b[5 + half]],
                       [selT_b])
                mk_idx = {}
                idx = 0
                for I in range(4):
                    for J in range(4 * I + 4):
                        col0 = max(0, 128 * J - 512 * I)
                        ncol = 512 - col0
                        q0 = 512 * I + col0
                        sb_i = 4 + rr["s"] % 4
                        rr["s"] += 1
                        mm(S, G.psum[sb_i][:, 0:ncol], esel[:, 128 * J:128 * J + 128], selT[:, q0:q0 + ncol], True,
                           True, [esel_b, selT_b], [G.psum_b[sb_i]])
                        cp(S, "act" if idx % 2 == 0 else "dve", mk_all[:, idx, 0:ncol], G.psum[sb_i][:, 0:ncol],
                           [G.psum_b[sb_i]], [mk_all_b])
                        if J >= 4 * I:
                            tt(S, "dve", mk_all[:, idx, 0:128], mk_all[:, idx, 0:128], G.tri[:], ALU.mult,
                               [mk_all_b, G.cb], [mk_all_b])
                        mk_idx[(I, J)] = idx
                        idx += 1
                items = []
                for g in range(4):
                    hd = 4 * h + g
                    for I in range(4):
                        for br in (1, 2):
                            if br == 1:
                                tiles = []
                                for J in range(4 * I + 4):
                                    col0 = max(0, 128 * J - 512 * I)
                                    tiles.append((J, col0, mk_all[:, mk_idx[(I, J)], 0:512 - col0], mk_all_b))
                                kT, kT_b, vv, vv_b = ksT, ksT_b, vs_sb, vs_b
                            else:
                                tiles = []
                                order = [4 * I - 1, 4 * I - 4, 4 * I - 3, 4 * I - 2, 4 * I, 4 * I + 1, 4 * I + 2, 4 * I + 3]
                                if I == 0:
                                    order = [0, 1, 2, 3]
                                for J in order:
                                    rel = J - 4 * I
                                    if rel < 0:
                                        r_ = rel + 4
                                        tiles.append((J, 0, ("lo", 128 * r_, 128 * (r_ + 1)), None))
                                    else:
                                        tiles.append((J, 128 * rel, ("hi", 128 * rel, 512), None))
                                kT, kT_b, vv, vv_b = kwT, kwT_b, vw_sb, vw_b
                            grp = {}
                            for ti, (J, col0, mk, mk_b) in enumerate(tiles):
                                st = {}

                                def stage1(g=g, I=I, J=J, col0=col0, st=st, kT=kT, kT_b=kT_b, mk=mk):
                                    ncol = 512 - col0
                                    if isinstance(mk, tuple):
                                        ncol = mk[2] - col0
                                    q0 = 512 * I + col0
                                    st["sb"] = 4 + rr["s"] % 4
                                    rr["s"] += 1
                                    sb_i = st["sb"]
                                    mm(S, G.psum[sb_i][:, 0:ncol], kT[:, 128 * J:128 * J + 128],
                                       qT4[:, g, q0:q0 + ncol], True, True, [kT_b, qT4_b], [G.psum_b[sb_i]])

                                def stage2(g=g, hd=hd, I=I, J=J, br=br, col0=col0, mk=mk, mk_b=mk_b, st=st, ti=ti,
                                           nt=len(tiles), vv=vv, vv_b=vv_b, grp=grp):
                                    sl = slice(512 * I, 512 * I + 512)
                                    ncol = 512 - col0
                                    if ti == 0:
                                        grp["ob"], grp["db"] = rr["a"] % 2, 2 + rr["a"] % 2
                                        rr["a"] += 1
                                        grp["gt"] = gate_tile(hd * 3 + br, sl)
                                    ob, db = grp["ob"], grp["db"]
                                    sb_i = st["sb"]
                                    eb, eb_b = ebs[rr["eb"] % 3]
                                    rr["eb"] += 1
                                    pT, pT_b = pTs[rr["p"] % 5]
                                    rr["p"] += 1
                                    c1 = 512
                                    if isinstance(mk, tuple):
                                        kind_, b0, c1 = mk
                                        ncol = c1 - col0
                                        act(S, pT[:, 0:ncol], G.psum[sb_i][:, 0:ncol], AF.Exp, [G.psum_b[sb_i]], [pT_b],
                                            scale=DSCALE)
                                        if kind_ == "lo":
                                            tt(S, "dve", pT[:, b0:b0 + 128], pT[:, b0:b0 + 128], trilo[:], ALU.mult,
                                               [pT_b, trilo_b], [pT_b])
                                        else:
                                            tt(S, "dve", pT[:, 0:128], pT[:, 0:128], G.tri[:], ALU.mult, [pT_b, G.cb],
                                               [pT_b])
                                    else:
                                        act(S, eb[:, 0:ncol], G.psum[sb_i][:, 0:ncol], AF.Exp, [G.psum_b[sb_i]], [eb_b],
                                            scale=DSCALE)
                                        tt(S, "dve", pT[:, 0:ncol], eb[:, 0:ncol], mk, ALU.mult, [eb_b, mk_b], [pT_b])
                                    mm(S, G.psum[ob][:, col0:c1], vv[:, J, :], pT[:, 0:ncol], ti == 0, ti == nt - 1,
                                       [vv_b, pT_b], [G.psum_b[ob]])
                                    mm(S, G.psum[db][:, col0:c1], G.ones_bf[:], pT[:, 0:ncol], ti == 0, ti == nt - 1,
                                       [G.cb, pT_b], [G.psum_b[db]])

                                def stage3(g=g, I=I, grp=grp):
                                    sl = slice(512 * I, 512 * I + 512)
                                    ob, db = grp["ob"], grp["db"]
                                    gt, gt_b = grp["gt"]
                                    rden, rden_b = rdens[rr["r"] % 3]
                                    rr["r"] += 1
                                    t1, t1_b = t1s[rr["t"] % 3]
                                    rr["t"] += 1
                                    act(S, rden[:], G.psum[db][:], AF.Ln, [G.psum_b[db]], [rden_b])
                                    act(S, rden[:], rden[:], AF.Exp, [rden_b], [rden_b], scale=-1.0)
                                    tt(S, "dve", t1[:], G.psum[ob][:], rden[:], ALU.mult, [G.psum_b[ob], rden_b], [t1_b])
                                    tt(S, "dve", t1[:], t1[:], gt[:], ALU.mult, [t1_b, gt_b], [t1_b])
                                    tt(S, "dve", acc[:, g, sl], acc[:, g, sl], t1[:], ALU.add, [acc_b, t1_b], [acc_b])

                                items.append((stage1, stage2, stage3 if ti == len(tiles) - 1 else None))
                run_pipeline(items, 3)
                for g in range(4):
                    dma(S, "pool", G.nsaraw[4 * h + g], acc[:, g, :], [acc_b], [G.nsaraw_b], acc_b)
            S.end_phase()

        with ExitStack() as es4:
            nnorm, nnorm_b = cx.sb(es4, [128, 8], F32, "nnorm")
            dma(S, "sp", nnorm[:], G.nnorm[l], [], [nnorm_b], nnorm_b)
            raws = [cx.sb(es4, [128, 8, 512], F32, "raw8_%d" % i) for i in range(2)]
            mos = [cx.sb(es4, [128, 8, 512], BF16, "mo8_%d" % i) for i in range(2)]
            sq, sq_b = cx.sb(es4, [128, 512], BF16, "nsq")
            rstd, rstd_b = cx.sb(es4, [128, 512], F32, "nrstd")
            for I in range(4):
                sl = slice(512 * I, 512 * I + 512)
                raw, raw_b = raws[I % 2]
                mo8, mo8_b = mos[I % 2]
                dma(S, "sp", raw[:], G.nsaraw[:, :, sl].rearrange("h p t -> p h t"), [G.nsaraw_b], [raw_b], raw_b)
                for hd in range(8):
                    act(S, sq[:], raw[:, hd, :], AF.Square, [raw_b], [sq_b])
                    mm(S, G.psum[I % 2][:], G.ones_bf[:], sq[:], hd == 0, hd == 7, [G.cb, sq_b], [G.psum_b[I % 2]])
                act(S, rstd[:], G.psum[I % 2][:], AF.Ln, [G.psum_b[I % 2]], [rstd_b], scale=1.0 / 1024, bias=EPS)
                act(S, rstd[:], rstd[:], AF.Exp, [rstd_b], [rstd_b], scale=-0.5)
                for hd in range(8):
                    stt(S, mo8[:, hd, :], raw[:, hd, :], nnorm[:, hd:hd + 1], rstd[:], ALU.mult, ALU.mult,
                        [raw_b, nnorm_b, rstd_b], [mo8_b])
                dma(S, "pool", G.mix[8:16, :, sl].rearrange("h p t -> p h t"), mo8[:], [mo8_b], [G.mix_b], mo8_b)
            S.end_phase()
def phase_outproj(cx, l, mixT, mixT_b, x_cur, x_cur_b, x_nxt, x_nxt_b, G):
    S = cx.S
    W = G.w_out[l]
    with ExitStack() as es:
        ws = WStream(cx, es, 4096, 2, 8192, 2)
        xins = [cx.sb(es, [128, 512], F32, "oxin%d" % i) for i in range(3)]
        xos = [cx.sb(es, [128, 512], F32, "oxo%d" % i) for i in range(3)]
        for k in range(16):
            dma(S, "sp", mixT[:, k, :], G.mix[k], [G.mix_b], [mixT_b], mixT_b)
        nxt = ws.load(W[:, 0:512], G.wbuf, 16, 512, q="act")
        c = 0
        for p in range(4):
            wv, wv_b = nxt
            if p + 1 < 4:
                nxt = ws.load(W[:, 512 * (p + 1):512 * (p + 2)], G.wbuf, 16, 512, q="act")
            for i in range(NT):
                xin, xin_b = xins[c % 3]
                xo, xo_b = xos[c % 3]
                b = c % 8
                c += 1
                dma(S, "sp", xin[:], x_cur[128 * i:128 * i + 128, 512 * p:512 * p + 512], [x_cur_b], [xin_b], xin_b)
                for k in range(16):
                    mm(S, G.psum[b][:], mixT[:, k, 128 * i:128 * i + 128], wv[:, k, :], k == 0, k == 15,
                       [mixT_b, wv_b(k)], [G.psum_b[b]])
                tt(S, "dve", xo[:], G.psum[b][:], xin[:], ALU.add, [G.psum_b[b], xin_b], [xo_b])
                dma(S, "pool", x_nxt[128 * i:128 * i + 128, 512 * p:512 * p + 512], xo[:], [xo_b], [x_nxt_b], xo_b)
        S.end_phase(ws.bufs() + [mixT_b, x_nxt_b] + G.psum_b)


def phase_ffn_up(cx, l, hT, hT_b, G):
    S = cx.S
    W = G.w_up[l]
    with ExitStack() as es:
        ws = WStream(cx, es, 4096, 2, 4096, 4)
        fcw, fcw_b = cx.sb(es, [128, 88, 3], F32, "fcw")
        fcb, fcb_b = cx.sb(es, [128, 88], F32, "fcb")
        dma(S, "sp", fcw[:], G.ffn_cw[l], [], [fcw_b], fcw_b)
        dma(S, "sp", fcb[:], G.ffn_cb[l], [], [fcb_b], fcb_b)
        sgs = [cx.sb(es, [128, S_LEN + 4], F32, "fsg%d" % i) for i in range(2)]
        sus = [cx.sb(es, [128, S_LEN + 4], F32, "fsu%d" % i) for i in range(2)]
        yg, yg_b = cx.sb(es, [128, S_LEN], F32, "fyg")
        yu, yu_b = cx.sb(es, [128, S_LEN], F32, "fyu")
        gos = [cx.sb(es, [128, S_LEN], BF16, "fgo%d" % i) for i in range(2)]
        for t_, b_ in sgs + sus:
            S.add("pool", lambda e, t_=t_: e.memset(t_[:, 0:4], 0.0), [], [b_])

        def loadpair(pp):
            a = ws.load(W[:, 256 * pp:256 * pp + 256], G.wbuf, 16, 256)
            b = ws.load(W[:, D_FF + 256 * pp:D_FF + 256 * pp + 256], G.wbuf, 16, 256)
            return a, b

        nxt = loadpair(0)
        cc = 0
        grp = 0
        for pp in range(22):
            (wg, wg_b), (wu, wu_b) = nxt
            if pp + 1 < 22:
                nxt = loadpair(pp + 1)
            for m in range(2):
                c = 2 * pp + m
                sg, sg_b = sgs[cc % 2]
                su, su_b = sus[cc % 2]
                go, go_b = gos[cc % 2]
                cc += 1
                for half in range(2):
                    base = 4 * (grp % 2)
                    grp += 1
                    for k in range(16):
                        for n2 in range(2):
                            tsl = slice(1024 * half + 512 * n2, 1024 * half + 512 * n2 + 512)
                            mm(S, G.psum[base + n2][:], wg[:, k, 128 * m:128 * m + 128], hT[:, k, tsl], k == 0, k == 15,
                               [wg_b(k), hT_b], [G.psum_b[base + n2]])
                            mm(S, G.psum[base + 2 + n2][:], wu[:, k, 128 * m:128 * m + 128], hT[:, k, tsl], k == 0,
                               k == 15, [wu_b(k), hT_b], [G.psum_b[base + 2 + n2]])
                    for n2 in range(2):
                        o0 = 4 + 1024 * half + 512 * n2
                        cp(S, "act", sg[:, o0:o0 + 512], G.psum[base + n2][:], [G.psum_b[base + n2]], [sg_b])
                        cp(S, "act", su[:, o0:o0 + 512], G.psum[base + 2 + n2][:], [G.psum_b[base + 2 + n2]], [su_b])
                for (st_, sb_, y, y_b, ch) in ((sg, sg_b, yg, yg_b, c), (su, su_b, yu, yu_b, 44 + c)):
                    ts(S, "dve", y[:], st_[:, 4:4 + S_LEN], fcw[:, ch, 2:3], fcb[:, ch:ch + 1], ALU.mult, ALU.add,
                       [sb_, fcw_b, fcb_b], [y_b])
                    stt(S, y[:], st_[:, 3:3 + S_LEN], fcw[:, ch, 1:2], y[:], ALU.mult, ALU.add, [sb_, fcw_b, y_b], [y_b])
                    stt(S, y[:], st_[:, 2:2 + S_LEN], fcw[:, ch, 0:1], y[:], ALU.mult, ALU.add, [sb_, fcw_b, y_b], [y_b])
                act(S, yg[:], yg[:], AF.Silu, [yg_b], [yg_b])
                tt(S, "dve", go[:], yg[:], yu[:], ALU.mult, [yg_b, yu_b], [go_b])
                dma(S, "pool", G.gT[:, :, c, :].rearrange("b p t -> p b t"), go[:].rearrange("p (b t) -> p b t", b=8), [go_b],
                    [G.gT_b], go_b)
        S.end_phase(ws.bufs() + [yg_b, yu_b, G.gT_b] + G.psum_b)


def phase_ffn_down(cx, l, x_cur, x_cur_b, x_nxt, x_nxt_b, G):
    S = cx.S
    W = G.w_down[l]
    NC = 512
    with ExitStack() as es:
        ws = WStream(cx, es, 4096, 2, 44 * NC, 2)
        gbs = [cx.sb(es, [128, 44, 256], BF16, "gblk%d" % i) for i in range(2)]
        xins = [cx.sb(es, [128, NC], F32, "dxin%d" % i) for i in range(3)]
        xos = [cx.sb(es, [128, NC], F32, "dxo%d" % i) for i in range(3)]
        nxt = ws.load(W[:, 0:NC], G.wbuf, 44, NC, q="act")
        c = 0
        gc = 0
        for p in range(D // NC):
            wv, wv_b = nxt
            if p + 1 < D // NC:
                nxt = ws.load(W[:, NC * (p + 1):NC * (p + 2)], G.wbuf, 44, NC, q="act")
            for tb in range(8):
                gb, gb_b = gbs[gc % 2]
                gc += 1
                dma(S, "sp", gb[:], G.gT[tb], [G.gT_b], [gb_b], gb_b)
                for ti in range(2):
                    i = 2 * tb + ti
                    xin, xin_b = xins[c % 3]
                    xo, xo_b = xos[c % 3]
                    b = c % 8
                    c += 1
                    dma(S, "sp", xin[:], x_cur[128 * i:128 * i + 128, NC * p:NC * p + NC], [x_cur_b], [xin_b], xin_b)
                    for k in range(44):
                        mm(S, G.psum[b][:, 0:NC], gb[:, k, 128 * ti:128 * ti + 128], wv[:, k, :], k == 0, k == 43,
                           [gb_b, wv_b(k)], [G.psum_b[b]])
                    tt(S, "dve", xo[:], G.psum[b][:, 0:NC], xin[:], ALU.add, [G.psum_b[b], xin_b], [xo_b])
                    dma(S, "pool", x_nxt[128 * i:128 * i + 128, NC * p:NC * p + NC], xo[:], [xo_b], [x_nxt_b], xo_b)
        S.end_phase(ws.bufs() + [x_nxt_b] + G.psum_b)


def phase_final_norm(cx, x_ap, x_buf, gain_ap, out_ap, out_b, G):
    S = cx.S
    with ExitStack() as es:
        gb, gb_b = cx.sb(es, [128, D], F32, "fgb")
        xts = [cx.sb(es, [128, D], F32, "fxt%d" % i) for i in range(2)]
        ots = [cx.sb(es, [128, D], F32, "fot%d" % i) for i in range(2)]
        junk, junk_b = cx.sb(es, [128, D], BF16, "fjunk")
        st, st_b = cx.sb(es, [128, 4 * NT], F32, "fstats")
        dma(S, "sp", gb[:], gain_ap.partition_broadcast(128), [], [gb_b], gb_b)
        for i in range(NT):
            xt, xt_b = xts[i % 2]
            ot, ot_b = ots[i % 2]
            dma(S, "sp", xt[:], x_ap[i * 128:(i + 1) * 128, :], [x_buf], [xt_b], xt_b)
            ss = st[:, 4 * i:4 * i + 1]
            lnv = st[:, 4 * i + 1:4 * i + 2]
            rstd = st[:, 4 * i + 2:4 * i + 3]
            act(S, junk[:], xt[:], AF.Square, [xt_b], [junk_b, st_b], accum_out=ss)
            ts(S, "dve", lnv, ss, 1.0 / D, EPS, ALU.mult, ALU.add, [st_b], [st_b])
            act(S, lnv, lnv, AF.Ln, [st_b], [st_b])
            act(S, rstd, lnv, AF.Exp, [st_b], [st_b], scale=-0.5)
            stt(S, ot[:], xt[:], rstd, gb[:], ALU.mult, ALU.mult, [xt_b, st_b, gb_b], [ot_b])
            dma(S, "pool", out_ap[i * 128:(i + 1) * 128, :], ot[:], [ot_b], [out_b], ot_b)
        S.end_phase([junk_b, st_b, out_b])
def host_consts():
    bf = ml_dtypes.bfloat16
    c = {}
    c["ident_bf"] = np.eye(128, dtype=np.float32).astype(bf)
    c["ident_f"] = np.eye(128, dtype=np.float32)
    p = np.arange(128)
    c["tri"] = (p[:, None] <= p[None, :]).astype(np.float32).astype(bf)
    c["ones_bf"] = np.ones((128, 128), np.float32).astype(bf)
    pos = np.arange(S_LEN, dtype=np.float32)
    inv = (np.float32(500000.0) ** (-np.arange(0, 32, 2, dtype=np.float32) / np.float32(32))).astype(np.float32)
    ang = pos[None, :] * inv[:, None]
    cs, sn = np.cos(ang).astype(np.float32), np.sin(ang).astype(np.float32)
    c["ropeC"] = np.concatenate([cs, cs], 0)
    c["ropeS"] = np.concatenate([sn, sn], 0)
    r = np.zeros((128, 128), np.float32)
    for m in range(16):
        r[m + 16, m] = -1.0
        r[m, m + 16] = 1.0
    c["rot32"] = r.astype(bf)
    s8 = np.zeros((128, 8, 128), np.float32)
    for h in range(8):
        s8[h, h, :] = 1.0
    c["sel8"] = s8.reshape(128, 8 * 128)
    cc = np.arange(128)
    t = np.arange(S_LEN)
    c["cmask"] = ((cc[:, None] < 127) & (16 * cc[:, None] + 31 <= t[None, :])).astype(np.float32).astype(bf)
    j = np.arange(32)
    ov = (16 * cc[:, None] < 64 * (j[None, :] + 1)) & (16 * cc[:, None] + 32 > 64 * j[None, :]) & (cc[:, None] < 127)
    c["overlap"] = ov.astype(np.float32).astype(bf)
    tt_ = (128 * np.arange(16)[None, :, None] + p[:, None, None])
    cur = tt_ // 64
    jj = j[None, None, :]
    forced = (jj == 0) | (jj == cur) | (jj == cur - 1)
    valid = jj <= cur
    A = ((~forced) & valid).astype(np.float32)
    Bc = np.where(valid, np.where(forced, 1e4, 0.0), -1.0).astype(np.float32)
    c["impA"] = A.reshape(128, 512)
    c["impB"] = Bc.reshape(128, 512)
    E = np.zeros((128, 16, 128), np.float32)
    for J in range(16):
        for pp in range(128):
            E[2 * J + pp // 64, J, pp] = 1.0
    c["esel"] = E.reshape(128, 16 * 128).astype(bf)
    c["trilo"] = (p[None, :] < p[:, None]).astype(np.float32).astype(bf)
    return c


CONST_SPECS = [("ident_bf", [128, 128], BF16), ("ident_f", [128, 128], F32), ("tri", [128, 128], BF16),
               ("ones_bf", [128, 128], BF16), ("ropeC", [32, S_LEN], F32), ("ropeS", [32, S_LEN], F32),
               ("rot32", [128, 128], BF16), ("sel8", [128, 1024], F32),
               ("cmask", [128, S_LEN], BF16), ("overlap", [128, 32], BF16), ("impA", [128, 512], F32),
               ("impB", [128, 512], F32), ("esel", [128, 2048], BF16), ("trilo", [128, 128], BF16)]

PARAM_SPECS = [("attn_norm", [DEPTH, D]), ("ffn_norm", [DEPTH, D]), ("final_norm", [1, D]),
               ("w_in", [DEPTH, D, N_IN]), ("qk_cw", [DEPTH, 128, 16, 4]), ("qk_cb", [DEPTH, 128, 16]),
               ("gbias", [DEPTH, 16, 1]), ("mnorm", [DEPTH, 128, 8]), ("nnorm", [DEPTH, 128, 8]),
               ("pe_k", [DEPTH, 128, 32]), ("pe_v", [DEPTH, 128, 32]),
               ("cmp_w1_k", [DEPTH, 4096, 256]), ("cmp_w2_k", [DEPTH, 256, 128]),
               ("cmp_w1_v", [DEPTH, 4096, 256]), ("cmp_w2_v", [DEPTH, 256, 128]),
               ("w_out", [DEPTH, D, D]), ("w_up", [DEPTH, D, 2 * D_FF]), ("ffn_cw", [DEPTH, 128, 88, 3]),
               ("ffn_cb", [DEPTH, 128, 88]), ("w_down", [DEPTH, D_FF, D])]


def host_params(inp):
    f = lambda a: np.ascontiguousarray(np.asarray(a, dtype=np.float32))
    o = {}
    o["attn_norm"] = f(inp["attn_norm"])
    o["ffn_norm"] = f(inp["ffn_norm"])
    o["final_norm"] = f(inp["final_norm"]).reshape(1, D)
    o["w_in"] = f(inp["w_in"])
    o["qk_cw"] = f(np.asarray(inp["qk_conv_w"]).reshape(DEPTH, 4, 16, 128).transpose(0, 3, 2, 1))
    o["qk_cb"] = f(np.asarray(inp["qk_conv_b"]).reshape(DEPTH, 16, 128).transpose(0, 2, 1))
    o["gbias"] = f(np.concatenate([np.asarray(inp["i_bias"]), np.asarray(inp["f_bias"])], 1).reshape(DEPTH, 16, 1))
    o["mnorm"] = f(np.asarray(inp["mlstm_norm"]).reshape(DEPTH, 8, 128).transpose(0, 2, 1))
    o["nnorm"] = f(np.asarray(inp["nsa_norm"]).reshape(DEPTH, 8, 128).transpose(0, 2, 1))
    o["pe_k"] = f(np.asarray(inp["cmp_pe_k"]).transpose(0, 2, 1))
    o["pe_v"] = f(np.asarray(inp["cmp_pe_v"]).transpose(0, 2, 1))
    for k in ("cmp_w1_k", "cmp_w2_k", "cmp_w1_v", "cmp_w2_v", "w_out", "w_up", "w_down"):
        o[k] = f(inp[k])
    o["ffn_cw"] = f(np.asarray(inp["ffn_conv_w"]).reshape(DEPTH, 3, 88, 128).transpose(0, 3, 2, 1))
    o["ffn_cb"] = f(np.asarray(inp["ffn_conv_b"]).reshape(DEPTH, 88, 128).transpose(0, 2, 1))
    return o


class G_:
    pass


def build_program(nc, n_layers=DEPTH, debug=False, stop_after=None):
    with ExitStack() as es:
        cx = Ctx(nc, es)
        S = cx.S
        G = G_()
        kind = "ExternalOutput" if debug else "Internal"
        x_in = nc.dram_tensor("x", [S_LEN, D], F32, kind="ExternalInput").ap()
        x_in_b = Buf("x_in")
        for name, shape in PARAM_SPECS:
            setattr(G, name, nc.dram_tensor(name, shape, F32, kind="ExternalInput").ap())
        G.wbuf = Buf("weights")
        cin = {}
        for name, shape, dt_ in CONST_SPECS:
            cin[name] = nc.dram_tensor("c_" + name, shape, dt_, kind="ExternalInput").ap()
        G.ropeC_in, G.ropeS_in = cin["ropeC"], cin["ropeS"]
        G.cin = cin
        out = nc.dram_tensor("out", [S_LEN, D], F32, kind="ExternalOutput").ap()
        out_b = Buf("out")
        G.zfm, G.zfm_b = cx.dram("zfm", [N_ZCH, 128, S_LEN], BF16, kind)
        G.ztm, G.ztm_b = cx.dram("ztm", [S_LEN, 1536], BF16, kind)
        G.gif, G.gif_b = cx.dram("gif", [16, S_LEN], F32, kind)
        G.gng, G.gng_b = cx.dram("gng", [24, S_LEN], F32, kind)
        G.mix, G.mix_b = cx.dram("mix", [16, 128, S_LEN], BF16, kind)
        G.nsaraw, G.nsaraw_b = cx.dram("nsaraw", [8, 128, S_LEN], F32, kind)
        G.gT, G.gT_b = cx.dram("gT", [8, 128, 44, 256], BF16, kind)
        xa, xa_b = cx.dram("xa", [S_LEN, D], F32, kind)
        xb, xb_b = cx.dram("xb", [S_LEN, D], F32, kind)

        G.psum, G.psum_b = [], []
        for i in range(8):
            G.psum.append(es.enter_context(nc.psum_tensor("ps%d" % i, [128, 512], F32)))
            G.psum_b.append(Buf("ps%d" % i, excl=True))
        G.cb = Buf("consts")
        for name in ("ident_bf", "ident_f", "tri", "ones_bf", "rot32"):
            spec = [s for s in CONST_SPECS if s[0] == name][0]
            t, _ = cx.sb(es, spec[1], spec[2], name)
            dma(S, "sp", t[:], cin[name], [], [G.cb], G.cb)
            setattr(G, name, t)
        def done(phase):
            return stop_after is not None and stop_after == phase

        x_cur, x_cur_b = x_in, x_in_b
        finished = False
        for l in range(n_layers):
            with ExitStack() as eh:
                hT, hT_b = cx.sb(eh, [128, 16, S_LEN], BF16, "hT")
                phase_norm_T(cx, x_cur, x_cur_b, G.attn_norm[l], hT, hT_b, G)
                if done("norm1"):
                    finished = True
                    break
                phase_inproj(cx, l, hT, hT_b, G)
            if done("inproj"):
                finished = True
                break
            phase_mlstm(cx, l, G)
            if done("mlstm"):
                finished = True
                break
            phase_nsa(cx, l, G)
            if done("nsa"):
                finished = True
                break
            with ExitStack() as eh:
                hT, hT_b = cx.sb(eh, [128, 16, S_LEN], BF16, "mixT")
                phase_outproj(cx, l, hT, hT_b, x_cur, x_cur_b, xa, xa_b, G)
            if done("outproj"):
                finished = True
                break
            with ExitStack() as eh:
                hT, hT_b = cx.sb(eh, [128, 16, S_LEN], BF16, "h2T")
                phase_norm_T(cx, xa, xa_b, G.ffn_norm[l], hT, hT_b, G)
                phase_ffn_up(cx, l, hT, hT_b, G)
            if done("ffnup"):
                finished = True
                break
            phase_ffn_down(cx, l, xa, xa_b, xb, xb_b, G)
            x_cur, x_cur_b = xb, xb_b
        if not finished:
            phase_final_norm(cx, x_cur, x_cur_b, G.final_norm[0], out, out_b, G)
        S.emit()
    return nc


_CACHE = {}


def kernel(**inputs):
    x = np.asarray(inputs["x"], dtype=np.float32)
    hp = host_params(inputs)
    cs = host_consts()
    nc = bass.Bass("TRN2", target_bir_lowering=False)
    build_program(nc)
    base = dict(hp)
    base.update({"c_" + k: v for k, v in cs.items()})
    in_maps = []
    for b in range(8):
        m = dict(base)
        m["x"] = np.ascontiguousarray(x[b])
        in_maps.append(m)
    res = run_bass_kernel_spmd(nc, in_maps, core_ids=list(range(8)))
    out = np.stack([np.asarray(r["out"], dtype=np.float32) for r in res.results], axis=0)
    return out
```
